# Optimizing a Trainium2 kernel written in Bass

```python
import jax, jax.numpy as jnp
from jax import lax
import numpy as np

D_MODEL = 1024
BATCH = 2
SEQ = 16384
DEPTH = 4

CHUNK = 64
CONV_CH = 512
CONV_WIDTH = 3
GLA_HEADS = 4
GLA_DK = 64
GLA_DV = 128
GLA_KW = GLA_HEADS * GLA_DK
GLA_VW = GLA_HEADS * GLA_DV
GATE_RANK = 16
GATE_NORMALIZER = 16.0
GATE_LOG_MIN = -1.0
MIX_WIDTH = CONV_CH + GLA_VW
IN_WIDTH = 3 * CONV_CH + 2 * GLA_KW + 2 * GLA_VW + GATE_RANK
IN_SPLITS = (
    CONV_CH,
    2 * CONV_CH,
    3 * CONV_CH,
    3 * CONV_CH + GLA_KW,
    3 * CONV_CH + 2 * GLA_KW,
    3 * CONV_CH + 2 * GLA_KW + GLA_VW,
    3 * CONV_CH + 2 * GLA_KW + 2 * GLA_VW,
)
D_FF = 2816
N_EXPERTS = 8
TOP_K = 2
D_FF_EXPERT = 2816
N_DENSE = (DEPTH + 1) // 2
N_MOE = DEPTH // 2
DEEPNORM_ALPHA = (2.0 * DEPTH) ** 0.25
DEEPNORM_BETA = (8.0 * DEPTH) ** -0.25
LN_EPS = 1e-5
RMS_EPS = 1e-6

kernel_name = "hybrid_conv_gla_moe_deepnorm_adaln_trunk"


def _layernorm(x, g, b):
    xf = x.astype(jnp.float32)
    mu = jnp.mean(xf, axis=-1, keepdims=True)
    var = jnp.mean(jnp.square(xf - mu), axis=-1, keepdims=True)
    return ((xf - mu) * lax.rsqrt(var + LN_EPS) * g.astype(jnp.float32)
            + b.astype(jnp.float32)).astype(x.dtype)


def _causal_depthwise_conv(z, w):
    return lax.conv_general_dilated(
        z, w[:, None, :].astype(z.dtype), window_strides=(1,),
        padding=[(CONV_WIDTH - 1, 0)],
        dimension_numbers=("NWC", "WIO", "NWC"),
        feature_group_count=z.shape[-1])


def _gla_chunked(q, k, v, gk):
    bsz, seq = q.shape[0], q.shape[1]
    n_chunks = seq // CHUNK
    shp_k = (bsz, n_chunks, CHUNK, GLA_HEADS, GLA_DK)
    shp_v = (bsz, n_chunks, CHUNK, GLA_HEADS, GLA_DV)
    q = q.astype(jnp.float32).reshape(shp_k) * (GLA_DK ** -0.5)
    k = k.astype(jnp.float32).reshape(shp_k)
    v = v.astype(jnp.float32).reshape(shp_v)
    b = jnp.cumsum(gk.astype(jnp.float32).reshape(shp_k), axis=2)
    b_last = b[:, :, -1:]
    q_in = q * jnp.exp(b)
    k_in = k * jnp.exp(-b)
    k_out = k * jnp.exp(b_last - b)
    causal = jnp.tril(jnp.ones((CHUNK, CHUNK), dtype=bool))
    scores = jnp.einsum("bnihd,bnjhd->bnhij", q_in, k_in)
    scores = jnp.where(causal, scores, 0.0)
    o_intra = jnp.einsum("bnhij,bnjhv->bnihv", scores, v)
    kv_chunk = jnp.einsum("bnjhd,bnjhv->bnhdv", k_out, v)
    decay_chunk = jnp.exp(b_last[:, :, 0])

    def step(state, inp):
        kv_n, dec_n = inp
        return dec_n[..., None] * state + kv_n, state

    init = jnp.zeros((bsz, GLA_HEADS, GLA_DK, GLA_DV), jnp.float32)
    _, states = lax.scan(step, init, (jnp.swapaxes(kv_chunk, 0, 1), jnp.swapaxes(decay_chunk, 0, 1)))
    states = jnp.swapaxes(states, 0, 1)
    o_inter = jnp.einsum("bnihd,bnhdv->bnihv", q_in, states)
    return (o_intra + o_inter).reshape(bsz, seq, GLA_HEADS, GLA_DV)


def _mixer(u, w_in, conv_w, gk_w2, gk_b, gla_norm_w, w_out):
    bsz, seq, _ = u.shape
    proj = jnp.einsum("bsd,de->bse", u, w_in)
    c_b, c_c, c_h, q, k, v, g, g_low = jnp.split(proj, IN_SPLITS, axis=-1)
    y_conv = c_b * _causal_depthwise_conv(c_c * c_h, conv_w)
    gk = jax.nn.log_sigmoid((jnp.einsum("bsr,rk->bsk", g_low, gk_w2) + gk_b).astype(jnp.float32))
    gk = jnp.maximum(gk / GATE_NORMALIZER, GATE_LOG_MIN)
    o = _gla_chunked(q.reshape(bsz, seq, GLA_HEADS, GLA_DK),
                     k.reshape(bsz, seq, GLA_HEADS, GLA_DK),
                     v.reshape(bsz, seq, GLA_HEADS, GLA_DV),
                     gk.reshape(bsz, seq, GLA_HEADS, GLA_DK))
    o = o * lax.rsqrt(jnp.mean(jnp.square(o), axis=-1, keepdims=True) + RMS_EPS)
    o = o * gla_norm_w.astype(jnp.float32) * jax.nn.silu(
        g.astype(jnp.float32).reshape(bsz, seq, GLA_HEADS, GLA_DV))
    y_gla = o.reshape(bsz, seq, GLA_VW).astype(u.dtype)
    y = jnp.concatenate([y_conv, y_gla], axis=-1)
    return jnp.einsum("bse,ed->bsd", y, w_out)


def _dense_swiglu(u, w_gate, w_up, w_down):
    h = jax.nn.silu(jnp.einsum("bsd,df->bsf", u, w_gate)) * jnp.einsum("bsd,df->bsf", u, w_up)
    return jnp.einsum("bsf,fd->bsd", h, w_down)


def _moe_swiglu(u, w_router, w_gate, w_up, w_down):
    bsz, seq, dm = u.shape
    n_tok = bsz * seq
    xt = u.reshape(n_tok, dm)
    logits = jnp.dot(xt, w_router).astype(jnp.float32)
    top_logits, top_idx = lax.top_k(logits, TOP_K)
    top_w = jax.nn.softmax(top_logits, axis=-1).astype(u.dtype)
    flat_e = top_idx.reshape(-1)
    order = jnp.argsort(flat_e)
    tok = order // TOP_K
    group_sizes = jnp.bincount(flat_e, length=N_EXPERTS).astype(jnp.int32)
    xs = xt[tok]
    h = jax.nn.silu(lax.ragged_dot(xs, w_gate, group_sizes)) * lax.ragged_dot(xs, w_up, group_sizes)
    ys = lax.ragged_dot(h, w_down, group_sizes) * top_w.reshape(-1)[order][:, None]
    out = jax.ops.segment_sum(ys, tok, num_segments=n_tok)
    return out.reshape(bsz, seq, dm)


def setup_inputs(seed: int = 0) -> dict:
    key = jax.random.key(seed)
    ks = jax.random.split(key, 24)
    nrm = jax.random.normal
    f32 = jnp.float32
    x = nrm(ks[0], (BATCH, SEQ, D_MODEL), f32)
    c = nrm(ks[1], (BATCH, D_MODEL), f32)
    ada_w = nrm(ks[2], (DEPTH, D_MODEL, 6 * D_MODEL), f32) * (0.1 * D_MODEL ** -0.5)
    ada_b = nrm(ks[3], (DEPTH, 6 * D_MODEL), f32) * 0.02
    w_in = nrm(ks[4], (DEPTH, D_MODEL, IN_WIDTH), f32) * D_MODEL ** -0.5
    conv_w = nrm(ks[5], (DEPTH, CONV_WIDTH, CONV_CH), f32) * CONV_WIDTH ** -0.5
    gk_w2 = nrm(ks[6], (DEPTH, GATE_RANK, GLA_KW), f32) * GATE_RANK ** -0.5
    gk_b = nrm(ks[7], (DEPTH, GLA_KW), f32) * 0.02
    gla_norm_w = 1.0 + 0.02 * nrm(ks[8], (DEPTH, GLA_DV), f32)
    w_out = nrm(ks[9], (DEPTH, MIX_WIDTH, D_MODEL), f32) * (DEEPNORM_BETA * MIX_WIDTH ** -0.5)
    ln1_g = 1.0 + 0.02 * nrm(ks[10], (DEPTH, D_MODEL), f32)
    ln1_b = 0.02 * nrm(ks[11], (DEPTH, D_MODEL), f32)
    ln2_g = 1.0 + 0.02 * nrm(ks[12], (DEPTH, D_MODEL), f32)
    ln2_b = 0.02 * nrm(ks[13], (DEPTH, D_MODEL), f32)
    ffn_w_gate = nrm(ks[14], (N_DENSE, D_MODEL, D_FF), f32) * D_MODEL ** -0.5
    ffn_w_up = nrm(ks[15], (N_DENSE, D_MODEL, D_FF), f32) * D_MODEL ** -0.5
    ffn_w_down = nrm(ks[16], (N_DENSE, D_FF, D_MODEL), f32) * (DEEPNORM_BETA * D_FF ** -0.5)
    router_w = nrm(ks[17], (N_MOE, D_MODEL, N_EXPERTS), f32) * D_MODEL ** -0.5
    moe_w_gate = nrm(ks[18], (N_MOE, N_EXPERTS, D_MODEL, D_FF_EXPERT), f32) * D_MODEL ** -0.5
    moe_w_up = nrm(ks[19], (N_MOE, N_EXPERTS, D_MODEL, D_FF_EXPERT), f32) * D_MODEL ** -0.5
    moe_w_down = nrm(ks[20], (N_MOE, N_EXPERTS, D_FF_EXPERT, D_MODEL), f32) * (DEEPNORM_BETA * D_FF_EXPERT ** -0.5)
    return {"x": x, "c": c, "ada_w": ada_w, "ada_b": ada_b, "w_in": w_in, "conv_w": conv_w,
            "gk_w2": gk_w2, "gk_b": gk_b, "gla_norm_w": gla_norm_w, "w_out": w_out,
            "ln1_g": ln1_g, "ln1_b": ln1_b, "ln2_g": ln2_g, "ln2_b": ln2_b,
            "ffn_w_gate": ffn_w_gate, "ffn_w_up": ffn_w_up, "ffn_w_down": ffn_w_down,
            "router_w": router_w, "moe_w_gate": moe_w_gate, "moe_w_up": moe_w_up,
            "moe_w_down": moe_w_down}


def reference(x, c, ada_w, ada_b, w_in, conv_w, gk_w2, gk_b, gla_norm_w, w_out,
              ln1_g, ln1_b, ln2_g, ln2_b, ffn_w_gate, ffn_w_up, ffn_w_down,
              router_w, moe_w_gate, moe_w_up, moe_w_down):
    cond = jax.nn.silu(c)
    for layer in range(DEPTH):
        mod = (jnp.dot(cond, ada_w[layer]) + ada_b[layer])[:, None, :]
        sh1, sc1, g1, sh2, sc2, g2 = jnp.split(mod, 6, axis=-1)
        u = x * (1.0 + sc1) + sh1
        mix = _mixer(u, w_in[layer], conv_w[layer], gk_w2[layer], gk_b[layer],
                     gla_norm_w[layer], w_out[layer])
        x = _layernorm(DEEPNORM_ALPHA * x + (1.0 + g1) * mix, ln1_g[layer], ln1_b[layer])
        u = x * (1.0 + sc2) + sh2
        j = layer // 2
        if layer % 2 == 0:
            ff = _dense_swiglu(u, ffn_w_gate[j], ffn_w_up[j], ffn_w_down[j])
        else:
            ff = _moe_swiglu(u, router_w[j], moe_w_gate[j], moe_w_up[j], moe_w_down[j])
        x = _layernorm(DEEPNORM_ALPHA * x + (1.0 + g2) * ff, ln2_g[layer], ln2_b[layer])
    return x
```

```python
import numpy as np
import concourse.bass as bass
import concourse.mybir as mybir
from concourse.bass_utils import run_bass_kernel_spmd

F32 = mybir.dt.float32
BF16 = mybir.dt.bfloat16
AF = mybir.ActivationFunctionType
ALU = mybir.AluOpType
AX = mybir.AxisListType

NCORES = 8
NT_FULL = 4096
T = 512
D = 1024
DEPTH = 4
INW = 3088
DFF = 2816
NFC = DFF // 128
NEXP = 8
ALPHA = (2.0 * DEPTH) ** 0.25
LN_EPS = 1e-5 / (ALPHA * ALPHA)
RMS_EPS = 1e-6
EXW = 272


class Op:
    __slots__ = ("eng", "fn", "deps", "dma", "dkey", "signal", "val", "pos", "idx")


class Sched:
    ENGS = ("pe", "dve", "act", "pool", "sp")

    def __init__(self):
        self.ops = []
        self.lastw = {}
        self.readers = {}
        self.lastdma = {}

    def add(self, eng, fn, reads=(), writes=(), dkey=None):
        op = Op()
        op.eng, op.fn, op.dkey = eng, fn, dkey
        op.dma = dkey is not None
        op.signal = False
        op.val = 0
        op.idx = len(self.ops)
        deps = {}
        for r in reads:
            w = self.lastw.get(r)
            if w is not None:
                deps[w.idx] = w
        for r in writes:
            w = self.lastw.get(r)
            if w is not None:
                deps[w.idx] = w
            for rd in self.readers.get(r, ()):
                deps[rd.idx] = rd
        if op.dma:
            prev = self.lastdma.get(dkey)
            if prev is not None:
                deps[prev.idx] = prev
            self.lastdma[dkey] = op
        op.deps = list(deps.values())
        for r in reads:
            self.readers.setdefault(r, []).append(op)
        for r in writes:
            self.lastw[r] = op
            self.readers[r] = []
        self.ops.append(op)
        return op

    def marker(self, payload):
        op = Op()
        op.eng, op.fn, op.dkey, op.dma, op.signal, op.val, op.deps = "cc", payload, None, False, False, 0, []
        op.idx = len(self.ops)
        self.ops.append(op)

    def emit(self, nc, block_engines, sems, dsems):
        cnt = {e: 0 for e in self.ENGS}
        cnt["cc"] = 0
        for op in self.ops:
            op.pos = cnt[op.eng]
            cnt[op.eng] += 1
        for op in self.ops:
            for d in op.deps:
                if d.dma:
                    d.signal = True
                elif d.eng != op.eng:
                    d.signal = True
                elif d.eng != "pe":
                    d.signal = True
        for op in self.ops:
            if op.dma:
                op.signal = True
        sc = {e: 0 for e in self.ENGS}
        dc = {}
        for op in self.ops:
            if not op.signal:
                continue
            if op.dma:
                dc[op.dkey] = dc.get(op.dkey, 0) + 16
                op.val = dc[op.dkey]
            else:
                sc[op.eng] += 1
                op.val = sc[op.eng]
        self.final_dma = dict(dc)
        segs = [[]]
        for op in self.ops:
            if op.eng == "cc":
                segs.append(op)
                segs.append([])
            else:
                segs[-1].append(op)
        return segs

    def run_engine(self, eng_name, eng, ops, sems, dsems, final=False):
        waited = {}
        for op in ops:
            for d in op.deps:
                if d.dma:
                    key = ("d", d.dkey)
                    sem = dsems[d.dkey]
                else:
                    if d.eng == eng_name and eng_name == "pe":
                        continue
                    key = ("e", d.eng)
                    sem = sems[d.eng]
                if waited.get(key, 0) >= d.val:
                    continue
                waited[key] = d.val
                eng.wait_ge(sem, d.val)
            ins = op.fn(eng)
            if op.signal:
                if op.dma:
                    ins.then_inc(dsems[op.dkey], 16)
                else:
                    ins.then_inc(sems[op.eng], 1)
        if final:
            for k, v in self.final_dma.items():
                if waited.get(("d", k), 0) < v:
                    eng.wait_ge(dsems[k], v)


class Arena:
    def __init__(self, nc, base, limit):
        self.nc, self.base, self.limit = nc, base, limit
        self.off = base
        self.n = 0

    def reset(self, to=None):
        self.off = self.base if to is None else to

    def mark(self):
        return self.off

    def alloc(self, name, shape, dtype):
        esz = 4 if dtype == F32 else 2
        nbytes = esz
        for s in shape[1:]:
            nbytes *= s
        nbytes = (nbytes + 63) // 64 * 64
        assert self.off + nbytes <= self.limit, (name, self.off, nbytes, self.limit)
        self.n += 1
        t = self.nc.alloc_sbuf_tensor_at(f"{name}_{self.n}", list(shape), dtype, offset=self.off)
        self.off += nbytes
        return t


def build(nlayers=DEPTH, stage="full", ntile=8, ncores=NCORES, layer=None, dbg_cut=99):
    nc = bass.Bass("TRN2", target_bir_lowering=False)
    S = Sched()

    declared = {}

    class LazyIn:
        def __init__(self, name, shape):
            self.name, self.shape, self._ap = name, shape, None

        def ap(self):
            if self._ap is None:
                self._ap = nc.dram_tensor(self.name, list(self.shape), F32, kind="ExternalInput").ap()
                declared[self.name] = True
            return self._ap

        def __getitem__(self, k):
            return self.ap()[k]

    def dram_in(name, shape):
        return LazyIn(name, shape)

    NT = ntile * T
    NTILE = ntile
    x_in = dram_in("x", [NT, D])
    cvec = dram_in("cvec", [128, 8])
    phased_decl = stage in ("phA", "phB")
    LD = 1 if phased_decl else DEPTH
    LF = 1 if phased_decl else 2

    def LI(l):
        return 0 if phased_decl else l

    def FI(j):
        return 0 if phased_decl else j
    ada_w = dram_in("ada_w", [LD, D, 6 * D])
    ada_b = dram_in("ada_b", [128, DEPTH, 48])
    w_in = dram_in("w_in", [LD, D, INW])
    w_out = dram_in("w_out", [LD, D, D])
    pvec = dram_in("pvec", [128, DEPTH, 48])
    gkw = dram_in("gkw", [LD, 17, 256])
    ffn_wg = dram_in("ffn_w_gate", [LF, D, DFF])
    ffn_wu = dram_in("ffn_w_up", [LF, D, DFF])
    ffn_wd = dram_in("ffn_w_down", [LF, DFF, D])
    router_w = dram_in("router_w", [LF, D, NEXP])
    moe_wg = dram_in("moe_w_gate", [LF, NEXP, D, DFF])
    moe_wu = dram_in("moe_w_up", [LF, NEXP, D, DFF])
    moe_wd = dram_in("moe_w_down", [LF, NEXP, DFF, D])
    sel = dram_in("sel", [128, 16])
    consts = dram_in("consts", [128, 1024])
    _out_is_output = not (stage in ("phA", "phB") and not (stage == "phB" and layer == DEPTH - 1))
    out = nc.dram_tensor("out", [NT, D], F32, kind=("ExternalOutput" if _out_is_output else "Internal")).ap()
    phased = phased_decl
    layers = list(range(nlayers)) if layer is None else [layer]
    last_layer = (layer == DEPTH - 1)
    xs_exported = phased and not (stage == "phB" and last_layer)
    xs = nc.dram_tensor("xs", [8, 128, NT], F32, kind=("ExternalOutput" if xs_exported else "Internal")).ap()
    xs_in = dram_in("xs_in", [8, 128, NT])
    xs1 = nc.dram_tensor("xs1", [8, 128, NT], F32).ap()
    exs = nc.dram_tensor("exs", [128, EXW], F32, kind=("ExternalOutput" if stage == "phA" else "Internal")).ap()
    exg_in = dram_in("exg_in", [ncores * 128, EXW])
    exg = nc.dram_tensor("exg", [ncores * 128, EXW], F32, addr_space="Local").ap()

    SB_LIMIT = 190 * 1024
    ar = Arena(nc, 16640, 229376 - 128)

    cst = ar.alloc("cst", [128, 1024], F32)
    IDENT = cst[:, 0:128]
    TRIIND = cst[:, 128:258]
    SUF = cst[:, 384:512]
    MASK = cst[:, 512:640]
    onesD = ar.alloc("onesD", [128, 128], F32)
    onesV = ar.alloc("onesV", [128, 128], F32)
    ones1 = ar.alloc("ones1", [128, 128], F32)
    modp = ar.alloc("modp", [128, DEPTH, 48], F32)
    pv = ar.alloc("pv", [128, DEPTH, 48], F32)
    selt = ar.alloc("selt", [128, 16], F32)
    condT = ar.alloc("condT", [128, 8], F32)
    epsln = ar.alloc("epsln", [128, 1], F32)
    epsrms = ar.alloc("epsrms", [128, 1], F32)
    bartile = ar.alloc("bartile", [128, 16], F32)
    Sin = [ar.alloc("Sin", [128, 128], F32) for _ in range(2)]
    zin = ar.alloc("zin", [128, 4, 2], F32)
    for p_ in range(2):
        S.add("dve", lambda e, p_=p_: e.memset(Sin[p_][:], 0.0), [], [("Sin", p_)])
    S.add("dve", lambda e: e.memset(zin[:], 0.0), [], ["zin"])
    persist_mark = ar.mark()

    ps = [nc.alloc_psum_tensor(f"ps{i}", [128, 512], F32) for i in range(8)]

    uid = [0]

    def fresh(prefix):
        uid[0] += 1
        return f"{prefix}{uid[0]}"

    def dma(q, out_ap, in_ap, reads, writes, dkey):
        S.add(q, lambda e, o=out_ap, i=in_ap: e.dma_start(out=o, in_=i), reads, writes, dkey=dkey)

    def barrier():
        allres = list(dict.fromkeys(list(S.lastw.keys()) + list(S.readers.keys())))
        tok = fresh("bar")
        S.add("dve", lambda e: e.memset(bartile[:], 0.0), reads=(), writes=allres + [tok])
        for q in ("pe", "act", "pool", "sp"):
            S.add(q, lambda e: e.nop(), reads=[tok], writes=[])

    dma("sp", cst[:], consts.ap(), [], ["cst"], "c0")
    dma("sp", pv[:], pvec.ap(), [], ["pv"], "c1")
    dma("sp", selt[:], sel.ap(), [], ["selt"], "c2")
    dma("sp", condT[:], cvec.ap(), [], ["condT"], "c3")
    dma("sp", modp[:], ada_b.ap(), [], ["modp"], "c4")
    S.add("dve", lambda e: e.memset(onesD[:], 1.0 / 1024.0), [], ["onesD"])
    S.add("dve", lambda e: e.memset(onesV[:], 1.0 / 128.0), [], ["onesV"])
    S.add("dve", lambda e: e.memset(ones1[:], 1.0), [], ["ones1"])
    S.add("dve", lambda e: e.memset(epsln[:], LN_EPS), [], ["epsln"])
    S.add("dve", lambda e: e.memset(epsrms[:], RMS_EPS), [], ["epsrms"])
    S.add("act", lambda e: e.activation(out=condT[:], in_=condT[:], func=AF.Silu), ["condT"], ["condT"])

    ABLK = 768
    m0 = ar.mark()
    adabuf = [ar.alloc("adabuf", [128, 8, ABLK], F32) for _ in range(2)]
    nb = 0
    for l in layers:
        for blk in range(6 * D // ABLK):
            slot = nb % 2
            nb += 1
            src = ada_w[LI(l), :, blk * ABLK:(blk + 1) * ABLK].rearrange("(c p) f -> p c f", p=128)
            dma("sp", adabuf[slot][:], src, [], [f"adabuf{slot}"], f"ada{slot}")
            for jj in range(ABLK // 128):
                j = blk * (ABLK // 128) + jj

                def mm(e, slot=slot, jj=jj, j=j, l=l):
                    ins = None
                    for k in range(8):
                        ins = e.matmul(ps[0][:, l * 48 + j:l * 48 + j + 1],
                                       lhsT=adabuf[slot][:, k, jj * 128:(jj + 1) * 128],
                                       rhs=condT[:, k:k + 1], start=(k == 0), stop=(k == 7))
                    return ins
                S.add("pe", mm, [f"adabuf{slot}", "condT"], ["ps0"])
    for l in layers:
        S.add("dve", lambda e, l=l: e.tensor_tensor(
            out=modp[:, l, :], in0=ps[0][:, l * 48:(l + 1) * 48], in1=modp[:, l, :], op=ALU.add), ["ps0", "modp"], ["modp"])
    for (lo, mul) in ((8, 1.0), (16, 1.0 / ALPHA), (32, 1.0), (40, 1.0 / ALPHA)):
        S.add("dve", lambda e, lo=lo, mul=mul: e.tensor_scalar(
            out=modp[:, :, lo:lo + 8], in0=modp[:, :, lo:lo + 8], scalar1=1.0, scalar2=mul,
            op0=ALU.add, op1=ALU.mult), ["modp"], ["modp"])
    barrier()
    ar.reset(m0)

    do_pre = (not phased) or (stage == "phA" and layer == 0)
    do_final = (not phased) or (stage == "phB" and last_layer)
    if phased and not do_pre:
        for t in range(NTILE):
            dma("sp", xs[:, :, t * T:(t + 1) * T], xs_in[:, :, t * T:(t + 1) * T], [], [("xs", t)], f"xsin{t % 2}")
        barrier()
    m0 = ar.mark()
    p_xin = [ar.alloc("xin", [128, D], F32) for _ in range(2)]
    p_xT = [ar.alloc("xT", [128, 8, T], F32) for _ in range(2)]
    for t in range(NTILE if do_pre else 0):
        ts = t % 2
        for g in range(4):
            gi = t * 4 + g
            sl = gi % 2
            dma("sp", p_xin[sl][:], x_in[gi * 128:(gi + 1) * 128, :], [], [f"xin{sl}"], f"xin{sl}")
            for half in range(2):
                bank = 1 + half

                def tr(e, sl=sl, half=half, bank=bank):
                    ins = None
                    for c in range(4):
                        cc = half * 4 + c
                        ins = e.transpose(out=ps[bank][:, c * 128:(c + 1) * 128],
                                          in_=p_xin[sl][:, cc * 128:(cc + 1) * 128], identity=IDENT)
                    return ins
                S.add("pe", tr, [f"xin{sl}", "cst"], [f"ps{bank}"])
                eng = "dve"

                def cp(e, ts=ts, half=half, bank=bank, g=g, eng=eng):
                    o = p_xT[ts][:, half * 4:(half + 1) * 4, g * 128:(g + 1) * 128]
                    i = ps[bank][:].rearrange("p (c t) -> p c t", c=4)
                    if eng == "act":
                        return e.activation(out=o, in_=i, func=AF.Copy)
                    return e.tensor_copy(out=o, in_=i)
                S.add(eng, cp, [f"ps{bank}"], [(f"xT{ts}", g, half)])
        rd = [(f"xT{ts}", g, h) for g in range(4) for h in range(2)]
        dma("sp", xs[:, :, t * T:(t + 1) * T].rearrange("c p t -> p c t"), p_xT[ts][:], rd, [("xs", t)], f"xsw{ts}")
    barrier()
    ar.reset(m0)


    def A1c(l, c):
        return modp[:, l, 8 + c:9 + c]

    def SH1c(l, c):
        return modp[:, l, 0 + c:1 + c]

    def G1c(l, c):
        return modp[:, l, 16 + c:17 + c]

    def SH2c(l, c):
        return modp[:, l, 24 + c:25 + c]

    def A2c(l, c):
        return modp[:, l, 32 + c:33 + c]

    def G2c(l, c):
        return modp[:, l, 40 + c:41 + c]

    def cast_load(dst, src, a, b, res, dkey):
        k = 0
        for c0 in (0, 4):
            for a0 in range(a, b, 1024):
                b0 = min(b, a0 + 1024)
                dma("pool", dst[:, c0:c0 + 4, a0:b0], src[:, c0:c0 + 4, a0:b0], [], [res], f"{dkey}_{k % 2}")
                k += 1

    def load_x_tile(xT, src, t, key):
        dma("sp", xT[:], src[:, :, t * T:(t + 1) * T].rearrange("c p t -> p c t"),
            [(key, t)], [("xT", j) for j in range(8)], "xTld")

    def store_x_tile(xT, dst, t, key):
        dma("sp", dst[:, :, t * T:(t + 1) * T].rearrange("c p t -> p c t"), xT[:],
            [("xT", j) for j in range(8)], [(key, t)], "xTst")

    def modulate(l, xT, u32, ub, first):
        for c in range(8):
            a, sh = (A1c(l, c), SH1c(l, c)) if first else (A2c(l, c), SH2c(l, c))
            if u32 is not None:
                S.add("dve", lambda e, c=c, a=a, sh=sh: e.tensor_scalar(
                    out=u32[:, c, :], in0=xT[:, c, :], scalar1=a, scalar2=sh, op0=ALU.mult, op1=ALU.add),
                    [("xT", c), "modp"], [("u32", c)])
            S.add("act", lambda e, c=c, a=a, sh=sh: e.activation(
                out=ub[:, c, :], in_=xT[:, c, :], func=AF.Identity, scale=a, bias=sh),
                [("xT", c), "modp"], [("ub", c)])

    def layernorm_tile(l, xT, rT, lnt, which):
        sq, mean, m2, rstd, nb, tmp = lnt
        goff = 0 if which == 1 else 16

        def stats(e):
            ins = None
            for j in range(8):
                ins = e.matmul(ps[2][:, :], lhsT=onesD[:], rhs=rT[:, j, :], start=(j == 0), stop=(j == 7))
            return ins
        S.add("pe", stats, [("u32", j) for j in range(8)] + ["onesD"], ["ps2"])
        for j in range(8):
            S.add("act", lambda e, j=j: e.activation(out=sq[j % 2][:], in_=rT[:, j, :], func=AF.Square),
                  [("u32", j)], [("sq", j % 2)])
            S.add("pe", lambda e, j=j: e.matmul(ps[3][:, :], lhsT=onesD[:], rhs=sq[j % 2][:], start=(j == 0), stop=(j == 7)),
                  [("sq", j % 2), "onesD"], ["ps3"])
        S.add("act", lambda e: e.activation(out=mean[:], in_=ps[2][:, :], func=AF.Copy), ["ps2"], ["mean"])
        S.add("dve", lambda e: e.tensor_tensor(out=m2[:], in0=mean[:], in1=mean[:], op=ALU.mult), ["mean"], ["m2"])
        S.add("dve", lambda e: e.tensor_tensor(out=m2[:], in0=ps[3][:, :], in1=m2[:], op=ALU.subtract), ["ps3", "m2"], ["m2"])
        S.add("act", lambda e: e.activation(out=rstd[:], in_=m2[:], func=AF.Sqrt, bias=epsln[:]), ["m2", "epsln"], ["rstd"])
        S.add("dve", lambda e: e.reciprocal(out=rstd[:], in_=rstd[:]), ["rstd"], ["rstd"])
        S.add("dve", lambda e: e.scalar_tensor_tensor(out=nb[:], in0=mean[:], scalar=-1.0, in1=rstd[:], op0=ALU.mult, op1=ALU.mult),
              ["mean", "rstd"], ["nb"])
        for j in range(8):
            S.add("pool", lambda e, j=j: e.tensor_tensor(out=tmp[j % 2][:], in0=rT[:, j, :], in1=rstd[:], op=ALU.mult),
                  [("u32", j), "rstd"], [("lntmp", j % 2)])
            S.add("dve", lambda e, j=j: e.tensor_tensor(out=tmp[j % 2][:], in0=tmp[j % 2][:], in1=nb[:], op=ALU.add),
                  [("lntmp", j % 2), "nb"], [("lntmp", j % 2)])
            S.add("act", lambda e, j=j: e.activation(out=xT[:, j, :], in_=tmp[j % 2][:], func=AF.Identity,
                                                    scale=pv[:, l, goff + j:goff + j + 1], bias=pv[:, l, goff + 8 + j:goff + 9 + j]),
                  [("lntmp", j % 2), "pv"], [("xT", j)])

    def alloc_ln():
        sq = [ar.alloc("lnsq", [128, T], F32) for _ in range(2)]
        mean = ar.alloc("lnmean", [128, T], F32)
        m2 = ar.alloc("lnm2", [128, T], F32)
        rstd = ar.alloc("lnrstd", [128, T], F32)
        nb = ar.alloc("lnnb", [128, T], F32)
        tmp = [ar.alloc("lntmp", [128, T], F32) for _ in range(2)]
        return (sq, mean, m2, rstd, nb, tmp)

    def combine_exchange(src):
        mc = ar.mark()
        exin = ar.alloc("exin", [128, ncores, EXW], F32)
        coef = ar.alloc("coef", [128, 2], F32)
        stmp = ar.alloc("stmp", [128, 128], F32)
        dma("sp", exin[:], src.rearrange("(r p) f -> p r f", p=128), [], ["exin"], "exin")
        for p in range(2):
            S.add("dve", lambda e, p=p: e.memset(Sin[p][:], 0.0), [], [("Sin", p)])
        S.add("dve", lambda e: e.memset(zin[:], 0.0), [], ["zin"])
        for r in range(ncores):
            S.add("dve", lambda e, r=r: e.tensor_scalar(out=coef[:], in0=exin[:, r, 256:258], scalar1=-1.0, scalar2=selt[:, r:r + 1],
                                                       op0=ALU.add, op1=ALU.mult), ["exin", "selt"], ["coef"])
            S.add("dve", lambda e: e.tensor_scalar(out=coef[:], in0=coef[:], scalar1=1.0, scalar2=None, op0=ALU.add), ["coef"], ["coef"])
            for p in range(2):
                S.add("dve", lambda e, r=r, p=p: e.tensor_scalar(out=stmp[:], in0=exin[:, r, p * 128:(p + 1) * 128], scalar1=selt[:, r:r + 1],
                                                                scalar2=None, op0=ALU.mult), ["exin", "selt"], ["stmp"])
                S.add("dve", lambda e, p=p: e.scalar_tensor_tensor(out=Sin[p][:], in0=Sin[p][:], scalar=coef[:, p:p + 1], in1=stmp[:],
                                                                  op0=ALU.mult, op1=ALU.add), [("Sin", p), "coef", "stmp"], [("Sin", p)])
            S.add("dve", lambda e, r=r: e.scalar_tensor_tensor(
                out=zin[:].rearrange("p c k -> p (c k)"), in0=exin[:, r, 258:266], scalar=selt[:, 8 + r:9 + r],
                in1=zin[:].rearrange("p c k -> p (c k)"), op0=ALU.mult, op1=ALU.add), ["exin", "selt", "zin"], ["zin"])
        barrier()
        ar.reset(mc)

    def pass_B1(l, use_exchange, a_only=False):
        m0 = ar.mark()
        win = ar.alloc("win", [128, 8, INW], BF16)
        wout = ar.alloc("wout", [128, 8, D], BF16)
        wgl = ar.alloc("wgl", [128, 8, 16], F32)
        gkwt = ar.alloc("gkwt", [17, 256], F32)
        xT = ar.alloc("xT", [128, 8, T], F32)
        u32 = ar.alloc("u32", [128, 8, T], F32)
        ub = ar.alloc("ub", [128, 8, T], BF16)
        ccs = [ar.alloc("ccs", [128, T], F32) for _ in range(2)]
        accs = [ar.alloc("accs", [128, T], F32) for _ in range(2)]
        z = ar.alloc("z", [128, 4, T + 2], F32)
        yT = ar.alloc("yT", [128, 8, T], BF16)
        qT = ar.alloc("qT", [128, 2, T], F32)
        kT = ar.alloc("kT", [128, 2, T], F32)
        sg = ar.alloc("sg", [128, 4, T], BF16)
        qin = ar.alloc("qin", [128, 2, T], BF16)
        kin = ar.alloc("kin", [128, 2, T], BF16)
        glT = ar.alloc("glT", [17, T], F32)
        t1 = ar.alloc("t1", [128, 256], F32)
        gkt = ar.alloc("gkt", [128, 256], F32)
        er = ar.alloc("er", [128, 256], F32)
        kout = ar.alloc("kout", [128, 256], BF16)
        vb = ar.alloc("vb", [128, 512], BF16)
        eb = ar.alloc("eb", [128, 2, 128], F32)
        enb = ar.alloc("enb", [128, 2, 128], F32)
        dect = ar.alloc("dect", [128, 2, 2], F32)
        AT = ar.alloc("AT", [128, 512], BF16)
        osq = ar.alloc("osq", [128, 512], F32)
        ors = ar.alloc("ors", [128, 512], F32)
        ot1 = ar.alloc("ot1", [128, 512], F32)
        mask4 = ar.alloc("mask4", [128, 512], F32)
        S32 = [ar.alloc("S32", [128, 128], F32) for _ in range(2)]
        SbH = [[ar.alloc("SbH", [128, 128], BF16) for _ in range(4)] for _ in range(4)]
        kinp = [ar.alloc("kinp", [128, 128], BF16) for _ in range(4)]
        lnt = alloc_ln()

        wsrc = w_in[LI(l)].rearrange("(c p) f -> p c f", p=128)
        for i, (a, b) in enumerate(((0, 1536), (1536, 2048), (2048, 2560), (2560, 3088))):
            cast_load(win, wsrc, a, b, ("win", i), f"win{i}")
        cast_load(wout, w_out[LI(l)].rearrange("(c p) f -> p c f", p=128), 0, D, "wout", "wout")
        dma("sp", wgl[:], wsrc[:, :, 3072:3088], [], ["wgl"], "wgl")
        dma("sp", gkwt[:], gkw[LI(l)], [], ["gkwt"], "gkwt")
        S.add("dve", lambda e: e.memset(glT[:], 1.0), [], ["glT"])
        for h in range(4):
            S.add("dve", lambda e, h=h: e.tensor_copy(out=mask4[:, h * 128:(h + 1) * 128], in_=MASK), ["cst"], ["mask4"])
        for p in range(2):
            S.add("dve", lambda e, p=p: e.memset(S32[p][:], 0.0), [], [("S32", p)])
        for h in range(4):
            S.add("dve", lambda e, h=h: e.memset(kinp[h][:], 0.0), [], [("kinp", h)])
            for v in range(4):
                S.add("dve", lambda e, h=h, v=v: e.memset(SbH[h][v][:], 0.0), [], [("SbH", h, v)])
        S.add("dve", lambda e: e.memset(z[:], 0.0), [], [("z", c) for c in range(4)])
        Dacc = ar.alloc("Dacc", [128, 2, 2], F32)
        exb = ar.alloc("exb", [128, EXW], F32)
        S.add("dve", lambda e: e.memset(Dacc[:], 1.0), [], ["Dacc"])
        if use_exchange and not a_only:
            for p in range(2):
                S.add("dve", lambda e, p=p: e.tensor_copy(out=S32[p][:], in_=Sin[p][:]), [("Sin", p)], [("S32", p)])
                for hh in range(2):
                    hs = slice(hh * 64, (hh + 1) * 64)
                    S.add("dve", lambda e, p=p, hh=hh, hs=hs: e.tensor_copy(out=SbH[p * 2 + hh][0][hs, :], in_=Sin[p][hs, :]),
                          [("Sin", p)], [("SbH", p * 2 + hh, 0)])
            S.add("dve", lambda e: e.tensor_copy(out=z[:, :, T:T + 2], in_=zin[:]), ["zin"], [("z", c) for c in range(4)])
        WIN_CONV, WIN_QK, WIN_V, WIN_G = ("win", 0), ("win", 1), ("win", 2), ("win", 3)
        rotc = [0]

        def rot():
            rotc[0] = (rotc[0] + 1) % 2
            return rotc[0]

        def proj_fm(col0, bank, wres):
            def f(e):
                ins = None
                for c in range(8):
                    ins = e.matmul(ps[bank][:, :], lhsT=win[:, c, col0:col0 + 128], rhs=ub[:, c, :], start=(c == 0), stop=(c == 7))
                return ins
            S.add("pe", f, [wres] + [("ub", c) for c in range(8)], [f"ps{bank}"])

        for t in range(NTILE):
            load_x_tile(xT, xs, t, "xs")
            modulate(l, xT, u32, ub, True)
            for c in range(4 if (not a_only or t == NTILE - 1) else 0):
                b1 = rot()
                proj_fm(512 + c * 128, b1, WIN_CONV)
                S.add("act", lambda e, c=c, b1=b1: e.activation(out=ccs[c % 2][:], in_=ps[b1][:, :], func=AF.Copy),
                      [f"ps{b1}"], [("ccs", c % 2)])
                b2 = rot()
                proj_fm(1024 + c * 128, b2, WIN_CONV)
                S.add("dve", lambda e, c=c: e.tensor_copy(out=z[:, c, 0:2], in_=z[:, c, T:T + 2]), [("z", c)], [("z", c)])
                S.add("dve", lambda e, c=c, b2=b2: e.tensor_tensor(out=z[:, c, 2:T + 2], in0=ps[b2][:, :], in1=ccs[c % 2][:], op=ALU.mult),
                      [f"ps{b2}", ("ccs", c % 2), ("z", c)], [("z", c)])
                if a_only:
                    continue
                S.add("act", lambda e, c=c: e.activation(out=accs[c % 2][:], in_=z[:, c, 2:T + 2], func=AF.Identity,
                                                        scale=pv[:, l, 32 + c * 3 + 2:32 + c * 3 + 3]),
                      [("z", c), "pv"], [("accs", c % 2)])
                for k in (1, 0):
                    S.add("dve", lambda e, c=c, k=k: e.scalar_tensor_tensor(
                        out=accs[c % 2][:], in0=z[:, c, k:k + T], scalar=pv[:, l, 32 + c * 3 + k:32 + c * 3 + k + 1],
                        in1=accs[c % 2][:], op0=ALU.mult, op1=ALU.add), [("z", c), ("accs", c % 2), "pv"], [("accs", c % 2)])
                b3 = rot()
                proj_fm(c * 128, b3, WIN_CONV)
                S.add("dve", lambda e, c=c, b3=b3: e.tensor_tensor(out=yT[:, c, :], in0=ps[b3][:, :], in1=accs[c % 2][:], op=ALU.mult),
                      [f"ps{b3}", ("accs", c % 2)], [("yT", c)])
            for p in range(0 if a_only else 2):
                b1 = rot()
                proj_fm(1536 + p * 128, b1, WIN_QK)
                S.add("act", lambda e, p=p, b1=b1: e.activation(out=qT[:, p, :], in_=ps[b1][:, :], func=AF.Copy, scale=0.125),
                      [f"ps{b1}"], [("qT", p)])
                b2 = rot()
                proj_fm(1792 + p * 128, b2, WIN_QK)
                S.add("act", lambda e, p=p, b2=b2: e.activation(out=kT[:, p, :], in_=ps[b2][:, :], func=AF.Copy),
                      [f"ps{b2}"], [("kT", p)])
            for h in range(0 if a_only else 4):
                b1 = rot()
                proj_fm(2560 + h * 128, b1, WIN_G)
                S.add("act", lambda e, h=h, b1=b1: e.activation(out=sg[:, h, :], in_=ps[b1][:, :], func=AF.Silu),
                      [f"ps{b1}"], [("sg", h)])
            if a_only and dbg_cut <= 1:
                continue
            b1 = rot()

            def glp(e, b1=b1):
                ins = None
                for c in range(8):
                    ins = e.matmul(ps[b1][0:16, :], lhsT=wgl[:, c, :], rhs=u32[:, c, :], start=(c == 0), stop=(c == 7))
                return ins
            S.add("pe", glp, ["wgl"] + [("u32", c) for c in range(8)], [f"ps{b1}"])
            S.add("dve", lambda e, b1=b1: e.tensor_copy(out=glT[0:16, :], in_=ps[b1][0:16, :]), [f"ps{b1}"], ["glT"])
            if a_only and dbg_cut <= 2:
                continue
            for g in range(4):
                gs = slice(g * 128, (g + 1) * 128)
                bk, bv = 0, 1

                def ktm(e, gs=gs):
                    ins = None
                    for c in range(8):
                        ins = e.matmul(ps[0][:, 0:256], lhsT=ub[:, c, gs], rhs=win[:, c, 1792:2048], start=(c == 0), stop=(c == 7))
                    return ins
                S.add("pe", ktm, [WIN_QK] + [("ub", c) for c in range(8)], ["ps0"])

                def vtm(e, gs=gs):
                    ins = None
                    for c in range(8):
                        ins = e.matmul(ps[1][:, :], lhsT=ub[:, c, gs], rhs=win[:, c, 2048:2560], start=(c == 0), stop=(c == 7))
                    return ins
                S.add("pe", vtm, [WIN_V] + [("ub", c) for c in range(8)], ["ps1"])
                S.add("pe", lambda e, gs=gs: e.matmul(ps[5][:, 0:256], lhsT=glT[0:17, gs], rhs=gkwt[0:17, :], start=True, stop=True),
                      ["glT", "gkwt"], ["ps5"])
                S.add("act", lambda e: e.activation(out=t1[:], in_=ps[5][:, 0:256], func=AF.Exp, scale=-1.0), ["ps5"], ["t1"])
                S.add("act", lambda e: e.activation(out=t1[:], in_=t1[:], func=AF.Ln, bias=1.0), ["t1"], ["t1"])
                S.add("dve", lambda e: e.tensor_scalar(out=gkt[:], in0=t1[:], scalar1=-1.0 / 16.0, scalar2=-1.0, op0=ALU.mult, op1=ALU.max),
                      ["t1"], ["gkt"])
                S.add("pe", lambda e: e.matmul(ps[5][:, 256:512], lhsT=SUF, rhs=gkt[:], start=True, stop=True), ["gkt", "cst"], ["ps5"])
                S.add("act", lambda e: e.activation(out=er[:], in_=ps[5][:, 256:512], func=AF.Exp), ["ps5"], ["er"])
                S.add("dve", lambda e: e.tensor_tensor(out=kout[:], in0=ps[0][:, 0:256], in1=er[:], op=ALU.mult), ["ps0", "er"], ["kout"])
                S.add("act", lambda e: e.activation(out=vb[:], in_=ps[1][:, :], func=AF.Copy), ["ps1"], ["vb"])
                for p in range(2):
                    S.add("pe", lambda e, p=p: e.matmul(ps[6][:, p * 130:(p + 1) * 130], lhsT=gkt[:, p * 128:(p + 1) * 128], rhs=TRIIND,
                                                       start=True, stop=True), ["gkt", "cst"], ["ps6"])
                for p in range(2):
                    if a_only:
                        S.add("act", lambda e, p=p: e.activation(out=dect[:, p, :], in_=ps[6][:, p * 130 + 128:p * 130 + 130], func=AF.Exp),
                              ["ps6"], [("dect", p)])
                        S.add("dve", lambda e, p=p: e.tensor_tensor(out=Dacc[:, p, :], in0=Dacc[:, p, :], in1=dect[:, p, :], op=ALU.mult),
                              ["Dacc", ("dect", p)], ["Dacc"])
                        continue
                    S.add("act", lambda e, p=p: e.activation(out=eb[:, p, :], in_=ps[6][:, p * 130:p * 130 + 128], func=AF.Exp),
                          ["ps6"], [("eb", p)])
                    S.add("act", lambda e, p=p: e.activation(out=enb[:, p, :], in_=ps[6][:, p * 130:p * 130 + 128], func=AF.Exp, scale=-1.0),
                          ["ps6"], [("enb", p)])
                    S.add("act", lambda e, p=p: e.activation(out=dect[:, p, :], in_=ps[6][:, p * 130 + 128:p * 130 + 130], func=AF.Exp),
                          ["ps6"], [("dect", p)])
                    S.add("dve", lambda e, p=p, gs=gs: e.tensor_tensor(out=qin[:, p, gs], in0=qT[:, p, gs], in1=eb[:, p, :], op=ALU.mult),
                          [("qT", p), ("eb", p)], [("qin", p)])
                    for hh in range(2):
                        hs = slice(hh * 64, (hh + 1) * 64)
                        S.add("dve", lambda e, p=p, gs=gs, hs=hs, hh=hh: e.tensor_tensor(
                            out=kinp[p * 2 + hh][hs, :], in0=kT[hs, p, gs], in1=enb[hs, p, :], op=ALU.mult),
                            [("kT", p), ("enb", p)], [("kinp", p * 2 + hh)])
                if a_only and dbg_cut <= 3:
                    continue
                for c2 in range(2):
                    n = (t * 4 + g) * 2 + c2
                    cs = slice(c2 * 64, (c2 + 1) * 64)
                    for p in range(2):
                        S.add("pe", lambda e, p=p, cs=cs: e.matmul(ps[7][:, p * 256:(p + 1) * 256], lhsT=kout[cs, p * 128:(p + 1) * 128],
                                                                   rhs=vb[cs, p * 256:(p + 1) * 256], start=True, stop=True),
                              ["kout", "vb"], ["ps7"])
                    for p in range(2):
                        for hh in range(2):
                            hs = slice(hh * 64, (hh + 1) * 64)
                            S.add("dve", lambda e, p=p, hh=hh, hs=hs, c2=c2: e.scalar_tensor_tensor(
                                out=S32[p][hs, :], in0=S32[p][hs, :], scalar=dect[hs, p, c2:c2 + 1],
                                in1=ps[7][hs, p * 256 + hh * 128:p * 256 + hh * 128 + 128], op0=ALU.mult, op1=ALU.add),
                                [("S32", p), ("dect", p), "ps7"], [("S32", p)])
                        for hh in range(0 if a_only else 2):
                            hs = slice(hh * 64, (hh + 1) * 64)
                            S.add("pool", lambda e, p=p, n=n, hh=hh, hs=hs: e.tensor_copy(
                                out=SbH[p * 2 + hh][(n + 1) % 4][hs, :], in_=S32[p][hs, :]),
                                [("S32", p)], [("SbH", p * 2 + hh, (n + 1) % 4)])
                if a_only:
                    continue
                for h in range(4):
                    p, hs = h // 2, slice((h % 2) * 64, (h % 2) * 64 + 64)
                    S.add("pe", lambda e, h=h, p=p, gs=gs: e.matmul(ps[3][:, h * 128:(h + 1) * 128], lhsT=kinp[h][:, :], rhs=qin[:, p, gs],
                                                                   start=True, stop=True), [("kinp", h), ("qin", p)], ["ps3"])
                S.add("dve", lambda e: e.tensor_tensor(out=AT[:], in0=ps[3][:, :], in1=mask4[:], op=ALU.mult),
                      ["ps3", "mask4"], ["AT"])
                for c2 in range(2):
                    n = (t * 4 + g) * 2 + c2
                    cs = slice(c2 * 64, (c2 + 1) * 64)
                    for h in range(4):
                        p, hs = h // 2, slice((h % 2) * 64, (h % 2) * 64 + 64)
                        oc = slice(h * 128 + c2 * 64, h * 128 + c2 * 64 + 64)

                        def om(e, h=h, p=p, oc=oc, n=n, g=g, c2=c2):
                            e.matmul(ps[4][:, oc], lhsT=vb[:, h * 128:(h + 1) * 128], rhs=AT[:, oc], start=True, stop=False)
                            return e.matmul(ps[4][:, oc], lhsT=SbH[h][n % 4][:, :],
                                            rhs=qin[:, p, g * 128 + c2 * 64:g * 128 + c2 * 64 + 64], start=False, stop=True)
                        S.add("pe", om, ["vb", "AT", ("SbH", h, n % 4), ("qin", p)], ["ps4"])
                S.add("act", lambda e: e.activation(out=osq[:], in_=ps[4][:, :], func=AF.Square), ["ps4"], ["osq"])
                S.add("pe", lambda e: e.matmul(ps[2][:, :], lhsT=onesV[:], rhs=osq[:], start=True, stop=True), ["osq", "onesV"], ["ps2"])
                S.add("act", lambda e: e.activation(out=ors[:], in_=ps[2][:, :], func=AF.Sqrt, bias=epsrms[:]), ["ps2", "epsrms"], ["ors"])
                S.add("dve", lambda e: e.reciprocal(out=ors[:], in_=ors[:]), ["ors"], ["ors"])
                S.add("dve", lambda e: e.tensor_tensor(out=ot1[:], in0=ps[4][:, :], in1=ors[:], op=ALU.mult), ["ps4", "ors"], ["ot1"])
                S.add("dve", lambda e, gs=gs: e.scalar_tensor_tensor(
                    out=yT[:, 4:8, gs], in0=ot1[:].rearrange("p (h t) -> p h t", h=4), scalar=pv[:, l, 44:45],
                    in1=sg[:, :, gs], op0=ALU.mult, op1=ALU.mult), ["ot1", "pv"] + [("sg", h) for h in range(4)], [("yT", 4 + h) for h in range(4)])
            if a_only:
                continue
            for j in range(8):
                b1 = rot()

                def op_(e, j=j, b1=b1):
                    ins = None
                    for c in range(8):
                        ins = e.matmul(ps[b1][:, :], lhsT=wout[:, c, j * 128:(j + 1) * 128], rhs=yT[:, c, :], start=(c == 0), stop=(c == 7))
                    return ins
                S.add("pe", op_, ["wout"] + [("yT", c) for c in range(8)], [f"ps{b1}"])
                S.add("dve", lambda e, j=j, b1=b1: e.scalar_tensor_tensor(
                    out=u32[:, j, :], in0=ps[b1][:, :], scalar=G1c(l, j), in1=xT[:, j, :], op0=ALU.mult, op1=ALU.add),
                    [f"ps{b1}", ("xT", j), "modp"], [("u32", j)])
            layernorm_tile(l, xT, u32, lnt, 1)
            store_x_tile(xT, xs1, t, "xs1")
        if a_only:
            S.add("dve", lambda e: e.memset(exb[:], 0.0), [], ["exb"])
            for p in range(2):
                S.add("dve", lambda e, p=p: e.tensor_copy(out=exb[:, p * 128:(p + 1) * 128], in_=S32[p][:]), [("S32", p), "exb"], ["exb"])
            S.add("dve", lambda e: e.tensor_tensor(out=exb[:, 256:258], in0=Dacc[:, :, 0], in1=Dacc[:, :, 1], op=ALU.mult),
                  ["Dacc", "exb"], ["exb"])
            S.add("dve", lambda e: e.tensor_copy(out=exb[:, 258:266].rearrange("p (c k) -> p c k", k=2), in_=z[:, :, T:T + 2]),
                  [("z", c) for c in range(4)] + ["exb"], ["exb"])
            dma("sp", exs, exb[:], ["exb"], ["exs"], "exs")
            if not phased:
                barrier()
                S.marker("allgather")
                combine_exchange(exg)
        barrier()
        ar.reset(m0)

    def pass_B2(l):
        moe = (l % 2 == 1)
        jj = l // 2
        NE = NEXP if moe else 1
        m0 = ar.mark()
        xT = ar.alloc("xT", [128, 8, T], F32)
        u32 = ar.alloc("u32", [128, 8, T], F32)
        ub = ar.alloc("ub", [128, 8, T], BF16)
        WG = [ar.alloc("WG", [128, 8, 512], BF16) for _ in range(2)]
        WU = [ar.alloc("WU", [128, 8, 512], BF16) for _ in range(2)]
        WD = ar.alloc("WD", [128, NFC, 512], BF16)
        hT = ar.alloc("hT", [128, NFC, T], BF16)
        sgt = [ar.alloc("sgt", [128, T], F32) for _ in range(2)]
        lnt = alloc_ln()
        if moe:
            acc = ar.alloc("acc", [128, 8, T], F32)
            wb = ar.alloc("wb", [128, NEXP, T], BF16)
            htmp = [ar.alloc("htmp", [128, T], F32) for _ in range(2)]
            DG = ar.alloc("DG", [128, NEXP, 128], F32)
            wr = ar.alloc("wr", [128, 8, NEXP], F32)
            lg = ar.alloc("lg", [128, NEXP], F32)
            m8 = ar.alloc("m8", [128, 8], F32)
            rt = ar.alloc("rt", [128, 8], F32)
            ge2 = ar.alloc("ge2", [128, NEXP], F32)
            eq1 = ar.alloc("eq1", [128, NEXP], F32)
            wtm = ar.alloc("wtm", [128, NEXP], F32)
            dma("sp", wr[:], router_w[FI(jj)].rearrange("(c p) e -> p c e", p=128), [], ["wr"], "wr")
        PIECES = ((0, 8), (8, 16), (16, NFC))
        cnt = [0]

        for t in range(NTILE):
            load_x_tile(xT, xs1, t, "xs1")
            modulate(l, xT, u32 if moe else None, ub, False)
            if moe:
                for g in range(4):
                    gs = slice(g * 128, (g + 1) * 128)

                    def lgm(e, gs=gs):
                        ins = None
                        for c in range(8):
                            ins = e.matmul(ps[7][:, 0:NEXP], lhsT=u32[:, c, gs], rhs=wr[:, c, :], start=(c == 0), stop=(c == 7))
                        return ins
                    S.add("pe", lgm, ["wr"] + [("u32", c) for c in range(8)], ["ps7"])
                    S.add("dve", lambda e: e.tensor_copy(out=lg[:], in_=ps[7][:, 0:NEXP]), ["ps7"], ["lg"])
                    S.add("dve", lambda e: e.max(out=m8[:], in_=lg[:]), ["lg"], ["m8"])
                    S.add("dve", lambda e: e.tensor_tensor(out=rt[:, 0:1], in0=m8[:, 1:2], in1=m8[:, 0:1], op=ALU.subtract), ["m8"], ["rt"])
                    S.add("act", lambda e: e.activation(out=rt[:, 1:2], in_=rt[:, 0:1], func=AF.Exp), ["rt"], ["rt"])
                    S.add("dve", lambda e: e.tensor_scalar(out=rt[:, 2:3], in0=rt[:, 1:2], scalar1=1.0, scalar2=None, op0=ALU.add), ["rt"], ["rt"])
                    S.add("dve", lambda e: e.reciprocal(out=rt[:, 2:3], in_=rt[:, 2:3]), ["rt"], ["rt"])
                    S.add("dve", lambda e: e.tensor_tensor(out=rt[:, 3:4], in0=rt[:, 1:2], in1=rt[:, 2:3], op=ALU.mult), ["rt"], ["rt"])
                    S.add("dve", lambda e: e.tensor_tensor(out=rt[:, 4:5], in0=rt[:, 2:3], in1=rt[:, 3:4], op=ALU.subtract), ["rt"], ["rt"])
                    S.add("dve", lambda e: e.tensor_scalar(out=ge2[:], in0=lg[:], scalar1=m8[:, 1:2], scalar2=rt[:, 3:4], op0=ALU.is_ge, op1=ALU.mult),
                          ["lg", "m8", "rt"], ["ge2"])
                    S.add("dve", lambda e: e.tensor_scalar(out=eq1[:], in0=lg[:], scalar1=m8[:, 0:1], scalar2=rt[:, 4:5], op0=ALU.is_ge, op1=ALU.mult),
                          ["lg", "m8", "rt"], ["eq1"])
                    S.add("dve", lambda e: e.tensor_tensor(out=wtm[:], in0=ge2[:], in1=eq1[:], op=ALU.add), ["ge2", "eq1"], ["wtm"])
                    for ex in range(NEXP):
                        S.add("dve", lambda e, ex=ex: e.tensor_scalar(out=DG[:, ex, :], in0=IDENT, scalar1=wtm[:, ex:ex + 1], scalar2=None, op0=ALU.mult),
                              ["wtm", "cst"], [("DG", ex)])
                    for half in range(2):
                        S.add("pe", lambda e, half=half: e.matmul(ps[6][:, :], lhsT=ones1[:], rhs=DG[:, half * 4:(half + 1) * 4, :].rearrange("p a b -> p (a b)"),
                                                                   start=True, stop=True), [("DG", half * 4 + i) for i in range(4)] + ["ones1"], ["ps6"])
                        S.add("act", lambda e, half=half, gs=gs: e.activation(out=wb[:, half * 4:(half + 1) * 4, gs],
                                                                            in_=ps[6][:, :].rearrange("p (a b) -> p a b", a=4), func=AF.Copy),
                              ["ps6"], [("wb", half * 4 + i) for i in range(4)])
            for ex in range(NE):
                if moe:
                    wg_src = moe_wg[FI(jj), ex].rearrange("(c p) f -> p c f", p=128)
                    wu_src = moe_wu[FI(jj), ex].rearrange("(c p) f -> p c f", p=128)
                    wd_src = moe_wd[FI(jj), ex].rearrange("(c p) f -> p c f", p=128)
                else:
                    wg_src = ffn_wg[FI(jj)].rearrange("(c p) f -> p c f", p=128)
                    wu_src = ffn_wu[FI(jj)].rearrange("(c p) f -> p c f", p=128)
                    wd_src = ffn_wd[FI(jj)].rearrange("(c p) f -> p c f", p=128)
                for blk in range(6):
                    c0 = blk * 512
                    ncol = min(512, DFF - c0)
                    sl = cnt[0] % 2
                    cnt[0] += 1
                    dma("pool", WG[sl][:, :, 0:ncol], wg_src[:, :, c0:c0 + ncol], [], [("WG", sl)], f"WG{sl}")
                    dma("pool", WU[sl][:, :, 0:ncol], wu_src[:, :, c0:c0 + ncol], [], [("WU", sl)], f"WU{sl}")
                    for f in range(ncol // 128):
                        fc = blk * 4 + f
                        bg, bu = fc % 2, 2 + fc % 2

                        def gm(e, sl=sl, f=f, bg=bg):
                            ins = None
                            for c in range(8):
                                ins = e.matmul(ps[bg][:, :], lhsT=WG[sl][:, c, f * 128:(f + 1) * 128], rhs=ub[:, c, :], start=(c == 0), stop=(c == 7))
                            return ins
                        S.add("pe", gm, [("WG", sl)] + [("ub", c) for c in range(8)], [f"ps{bg}"])

                        def um(e, sl=sl, f=f, bu=bu):
                            ins = None
                            for c in range(8):
                                ins = e.matmul(ps[bu][:, :], lhsT=WU[sl][:, c, f * 128:(f + 1) * 128], rhs=ub[:, c, :], start=(c == 0), stop=(c == 7))
                            return ins
                        S.add("pe", um, [("WU", sl)] + [("ub", c) for c in range(8)], [f"ps{bu}"])
                        S.add("act", lambda e, fc=fc, bg=bg: e.activation(out=sgt[fc % 2][:], in_=ps[bg][:, :], func=AF.Silu),
                              [f"ps{bg}"], [("sgt", fc % 2)])
                        if moe:
                            S.add("dve", lambda e, fc=fc, bu=bu: e.tensor_tensor(out=htmp[fc % 2][:], in0=ps[bu][:, :], in1=sgt[fc % 2][:], op=ALU.mult),
                                  [f"ps{bu}", ("sgt", fc % 2)], [("htmp", fc % 2)])
                            S.add("dve", lambda e, fc=fc, ex=ex: e.tensor_tensor(out=hT[:, fc, :], in0=htmp[fc % 2][:], in1=wb[:, ex, :], op=ALU.mult),
                                  [("htmp", fc % 2), ("wb", ex)], [("hT", fc)])
                        else:
                            S.add("dve", lambda e, fc=fc, bu=bu: e.tensor_tensor(out=hT[:, fc, :], in0=ps[bu][:, :], in1=sgt[fc % 2][:], op=ALU.mult),
                                  [f"ps{bu}", ("sgt", fc % 2)], [("hT", fc)])
                for hf in range(2):
                    for pi, (k0, k1) in enumerate(PIECES):
                        dma("pool", WD[:, k0:k1, :], wd_src[:, k0:k1, hf * 512:(hf + 1) * 512], [], [("WD", pi)], f"WD{pi}")

                    def dm(e):
                        ins = None
                        for k in range(NFC):
                            for f in range(4):
                                ins = e.matmul(ps[4 + f][:, :], lhsT=WD[:, k, f * 128:(f + 1) * 128], rhs=hT[:, k, :], start=(k == 0), stop=(k == NFC - 1))
                        return ins
                    S.add("pe", dm, [("WD", pi) for pi in range(3)] + [("hT", k) for k in range(NFC)], ["ps4", "ps5", "ps6", "ps7"])
                    for f in range(4):
                        jo = hf * 4 + f
                        if not moe:
                            S.add("dve", lambda e, f=f, jo=jo: e.scalar_tensor_tensor(
                                out=u32[:, jo, :], in0=ps[4 + f][:, :], scalar=G2c(l, jo), in1=xT[:, jo, :], op0=ALU.mult, op1=ALU.add),
                                [f"ps{4 + f}", ("xT", jo), "modp"], [("u32", jo)])
                        elif ex == 0:
                            S.add("act", lambda e, f=f, jo=jo: e.activation(out=acc[:, jo, :], in_=ps[4 + f][:, :], func=AF.Copy),
                                  [f"ps{4 + f}"], [("acc", jo)])
                        else:
                            S.add("dve", lambda e, f=f, jo=jo: e.tensor_tensor(out=acc[:, jo, :], in0=ps[4 + f][:, :], in1=acc[:, jo, :], op=ALU.add),
                                  [f"ps{4 + f}", ("acc", jo)], [("acc", jo)])
            if moe:
                for jo in range(8):
                    S.add("dve", lambda e, jo=jo: e.scalar_tensor_tensor(
                        out=u32[:, jo, :], in0=acc[:, jo, :], scalar=G2c(l, jo), in1=xT[:, jo, :], op0=ALU.mult, op1=ALU.add),
                        [("acc", jo), ("xT", jo), "modp"], [("u32", jo)])
            layernorm_tile(l, xT, u32, lnt, 2)
            store_x_tile(xT, xs, t, "xs")
        barrier()
        ar.reset(m0)

    if stage == "b2":
        pass_B1(0, False)
        pass_B2(0)
    if stage == "l2":
        for l in range(nlayers):
            pass_B1(l, False)
            pass_B2(l)
    if stage == "phA":
        pass_B1(layer, True, a_only=True)
    if stage == "phB":
        combine_exchange(exg_in.ap())
        pass_B1(layer, True)
        pass_B2(layer)
    if stage in ("full", "fullx"):
        for l in range(nlayers):
            pass_B1(l, True, a_only=True)
            pass_B1(l, True)
            if stage == "full":
                pass_B2(l)
        if stage == "fullx":
            for t in range(NTILE):
                dma("sp", xs[:, :, t * T:(t + 1) * T], xs1[:, :, t * T:(t + 1) * T], [("xs1", t)], [("xs", t)], "dbgcp")
            barrier()
    if stage == "moe":
        for t in range(NTILE):
            dma("sp", xs1[:, :, t * T:(t + 1) * T], xs[:, :, t * T:(t + 1) * T], [("xs", t)], [("xs1", t)], "dbgcp")
        barrier()
        pass_B2(1)
    if stage in ("b1",):
        pass_B1(0, False)
        for t in range(NTILE):
            dma("sp", xs[:, :, t * T:(t + 1) * T], xs1[:, :, t * T:(t + 1) * T], [("xs1", t)], [("xs", t)], "dbgcp")
        barrier()


    if stage == "pre_xs":
        dma("sp", out.rearrange("r (a t) -> (r a) t", a=2).rearrange("(c p) t -> c p t", p=128), xs, [("xs", t) for t in range(NTILE)], ["outdbg"], "dbgx")
    m0 = ar.mark()
    f_xT = [ar.alloc("fxT", [128, 8, T], F32) for _ in range(2)]
    f_xo = [ar.alloc("xo", [128, D], F32) for _ in range(2)]
    for t in range(NTILE if (stage != "pre_xs" and do_final) else 0):
        ts = t % 2
        dma("sp", f_xT[ts][:], xs[:, :, t * T:(t + 1) * T].rearrange("c p t -> p c t"), [("xs", t)], [f"fxT{ts}"], f"fxr{ts}")
        for g in range(4):
            gi = t * 4 + g
            sl = gi % 2
            for half in range(2):
                bank = 1 + half

                def tr(e, ts=ts, half=half, bank=bank, g=g):
                    ins = None
                    for c in range(4):
                        cc = half * 4 + c
                        ins = e.transpose(out=ps[bank][:, c * 128:(c + 1) * 128],
                                          in_=f_xT[ts][:, cc, g * 128:(g + 1) * 128], identity=IDENT)
                    return ins
                S.add("pe", tr, [f"fxT{ts}", "cst"], [f"ps{bank}"])
                eng = "dve"

                def cp(e, sl=sl, half=half, bank=bank, eng=eng):
                    o = f_xo[sl][:, half * 512:(half + 1) * 512]
                    if eng == "act":
                        return e.activation(out=o, in_=ps[bank][:], func=AF.Copy)
                    return e.tensor_copy(out=o, in_=ps[bank][:])
                S.add(eng, cp, [f"ps{bank}"], [(f"xo{sl}", half)])
            dma("sp", out[gi * 128:(gi + 1) * 128, :], f_xo[sl][:], [(f"xo{sl}", 0), (f"xo{sl}", 1)], [("out", gi)], f"xo{sl}")
    ar.reset(m0)

    dkeys = list(dict.fromkeys(op.dkey for op in S.ops if op.dma))
    from contextlib import ExitStack
    with ExitStack() as es:
        sems = {e: es.enter_context(nc.semaphore(f"s_{e}")) for e in Sched.ENGS}
        dsems = {k: es.enter_context(nc.semaphore(f"d_{k}")) for k in dkeys}
        ccsem = es.enter_context(nc.semaphore("ccsem"))
        segs = S.emit(nc, None, sems, dsems)
        nblocks = (len(segs) + 1) // 2
        cccount = 0
        for si in range(0, len(segs), 2):
            seg = segs[si]
            last = (si == len(segs) - 1)
            pe_ops = {e: [op for op in seg if op.eng == e] for e in Sched.ENGS}
            with nc.Block() as block:
                @block.tensor
                def _(e, pe_ops=pe_ops):
                    S.run_engine("pe", e, pe_ops["pe"], sems, dsems)

                @block.vector
                def _(e, pe_ops=pe_ops):
                    S.run_engine("dve", e, pe_ops["dve"], sems, dsems)

                @block.scalar
                def _(e, pe_ops=pe_ops):
                    S.run_engine("act", e, pe_ops["act"], sems, dsems)

                @block.gpsimd
                def _(e, pe_ops=pe_ops):
                    S.run_engine("pool", e, pe_ops["pool"], sems, dsems)

                @block.sync
                def _(e, pe_ops=pe_ops, last=last):
                    S.run_engine("sp", e, pe_ops["sp"], sems, dsems, final=last)
            if not last:
                cccount += 16
                nc.gpsimd.collective_compute("AllGather", ALU.bypass, replica_groups=[list(range(ncores))],
                                             ins=[exs], outs=[exg]).then_inc(ccsem, 16)
                nc.gpsimd.wait_ge(ccsem, cccount)
                nc.all_engine_barrier()
    nc._declared = list(declared.keys())
    return nc


def make_consts():
    c = np.zeros((128, 1024), np.float32)
    c[:, 0:128] = np.eye(128, dtype=np.float32)
    j = np.arange(128)[:, None]
    i = np.arange(128)[None, :]
    same = (j // 64) == (i // 64)
    c[:, 128:256] = (same & (j <= i)).astype(np.float32)
    c[:, 256] = (np.arange(128) < 64).astype(np.float32)
    c[:, 257] = (np.arange(128) >= 64).astype(np.float32)
    c[:, 384:512] = (same & (j > i)).astype(np.float32)
    c[:, 512:640] = (same & (j <= i)).astype(np.float32)
    return c


def prep_inputs(inp, layer=None):
    f = lambda a: np.ascontiguousarray(np.asarray(a, dtype=np.float32))
    x = f(inp["x"])
    maps = []
    ada_b = f(np.transpose(np.asarray(inp["ada_b"]).reshape(DEPTH, 48, 128), (2, 0, 1)))
    pv = np.zeros((128, DEPTH, 48), np.float32)
    for l in range(DEPTH):
        for i, nm in enumerate(("ln1_g", "ln1_b", "ln2_g", "ln2_b")):
            pv[:, l, i * 8:(i + 1) * 8] = np.asarray(inp[nm])[l].reshape(8, 128).T
        cw = np.asarray(inp["conv_w"])[l]
        for c in range(4):
            for k in range(3):
                pv[:, l, 32 + c * 3 + k] = cw[k, c * 128:(c + 1) * 128]
        pv[:, l, 44] = np.asarray(inp["gla_norm_w"])[l]
    gkw = f(np.concatenate([np.asarray(inp["gk_w2"]), np.asarray(inp["gk_b"])[:, None, :]], axis=1))
    consts = make_consts()
    shared = dict(
        ada_w=f(inp["ada_w"]), ada_b=ada_b, w_in=f(inp["w_in"]), w_out=f(inp["w_out"]), pvec=pv, gkw=gkw,
        ffn_w_gate=f(inp["ffn_w_gate"]), ffn_w_up=f(inp["ffn_w_up"]), ffn_w_down=f(inp["ffn_w_down"]),
        router_w=f(inp["router_w"]), moe_w_gate=f(inp["moe_w_gate"]), moe_w_up=f(inp["moe_w_up"]),
        moe_w_down=f(inp["moe_w_down"]), consts=consts)
    if layer is not None:
        j = layer // 2
        for k in ("ada_w", "w_in", "w_out", "gkw"):
            shared[k] = np.ascontiguousarray(shared[k][layer:layer + 1])
        for k in ("ffn_w_gate", "ffn_w_up", "ffn_w_down", "router_w", "moe_w_gate", "moe_w_up", "moe_w_down"):
            shared[k] = np.ascontiguousarray(shared[k][j:j + 1])
    cfull = np.asarray(inp["c"], dtype=np.float32)
    for core in range(NCORES):
        b, k = core // 4, core % 4
        m = dict(shared)
        m["x"] = np.ascontiguousarray(x[b, k * NT_FULL:(k + 1) * NT_FULL, :])
        m["cvec"] = np.ascontiguousarray(cfull[b].reshape(8, 128).T)
        s = np.zeros((128, 16), np.float32)
        for r in range(NCORES):
            if r // 4 == b and r < core:
                s[:, r] = 1.0
            if r // 4 == b and r == core - 1:
                s[:, 8 + r] = 1.0
        m["sel"] = s
        maps.append(m)
    return maps


_NC_CACHE = {}


def _phase_nc(stage, layer):
    key = (stage, layer)
    if key not in _NC_CACHE:
        _NC_CACHE[key] = build(stage=stage, layer=layer)
    return _NC_CACHE[key]


def _launch(nc, maps):
    maps = [{k: m[k] for k in nc._declared} for m in maps]
    res = run_bass_kernel_spmd(nc, maps, core_ids=list(range(NCORES)))
    return [{k: np.asarray(v) for k, v in r.items()} for r in res.results]


def kernel(**inputs):
    xs = None
    out = None
    for l in range(DEPTH):
        maps = prep_inputs(inputs, layer=l)
        if xs is not None:
            for c in range(NCORES):
                maps[c]["xs_in"] = xs[c]
        rA = _launch(_phase_nc("phA", l), maps)
        if l == 0:
            xs = [r["xs"] for r in rA]
        exg = np.concatenate([r["exs"] for r in rA], axis=0)
        for c in range(NCORES):
            maps[c]["xs_in"] = xs[c]
            maps[c]["exg_in"] = exg
        rB = _launch(_phase_nc("phB", l), maps)
        if l < DEPTH - 1:
            xs = [r["xs"] for r in rB]
        else:
            out = [r["out"] for r in rB]
    full = np.stack([np.concatenate(out[0:4], axis=0), np.concatenate(out[4:8], axis=0)], axis=0)
    return full.astype(np.float32)
```
